# Optimizing a Trainium2 kernel written in Bass

```python
import jax
import jax.numpy as jnp
from jax import lax
import numpy as np

D_MODEL = 1024
BATCH = 2
SEQ = 16384
DEPTH = 2

CHUNK = 64
HEAD_DIM = 64
QBLOCK = 128
ROPE_THETA = 10000.0
EPS = 1e-6
NEG = -1e30

A_HEADS = 8
IDX_HEADS = 4
IDX_DIM = 64
IDX_TOPK_MAX = 256
B_HEADS = 8
B_PAST_CHUNKS = 8
B_REL_CLIP = 256
C_HEADS = 8
D_HEADS = 8
D_KV_HEADS = 2
D_GROUP = D_HEADS // D_KV_HEADS
D_WINDOW = 128
D_WINDOW_CHUNKS = -(-D_WINDOW // CHUNK)
D_FF = 2816
CONV_WIDTH = 3

N_EVEN = (DEPTH + 1) // 2
N_ODD = DEPTH // 2

EVEN_SPLITS = (A_HEADS * HEAD_DIM, A_HEADS * HEAD_DIM, A_HEADS * HEAD_DIM,
               IDX_HEADS * IDX_DIM, IDX_DIM, IDX_HEADS,
               B_HEADS * HEAD_DIM, B_HEADS * HEAD_DIM, B_HEADS * HEAD_DIM)
ODD_SPLITS = (C_HEADS * HEAD_DIM, C_HEADS * HEAD_DIM, C_HEADS * HEAD_DIM, C_HEADS,
              D_HEADS * HEAD_DIM, D_KV_HEADS * HEAD_DIM, D_KV_HEADS * HEAD_DIM)
EVEN_IN = sum(EVEN_SPLITS)
ODD_IN = sum(ODD_SPLITS)
EVEN_OUT = (A_HEADS + B_HEADS) * HEAD_DIM
ODD_OUT = (C_HEADS + D_HEADS) * HEAD_DIM

kernel_name = 'hybrid_chunk_causal_dsa_relpos_fox_swa'


def _split(t, sizes):
    return jnp.split(t, np.cumsum(sizes)[:-1].tolist(), axis=-1)


def rms_norm(x, g):
    xf = x.astype(jnp.float32)
    y = xf * lax.rsqrt(jnp.mean(xf * xf, axis=-1, keepdims=True) + EPS)
    return (y * g.astype(jnp.float32)).astype(x.dtype)


def layer_norm(x, g, b):
    xf = x.astype(jnp.float32)
    xc = xf - jnp.mean(xf, axis=-1, keepdims=True)
    y = xc * lax.rsqrt(jnp.mean(xc * xc, axis=-1, keepdims=True) + EPS)
    return (y * g.astype(jnp.float32) + b.astype(jnp.float32)).astype(x.dtype)


def rope_tables(seq, dim):
    inv = 1.0 / (ROPE_THETA ** (jnp.arange(0, dim, 2, dtype=jnp.float32) / dim))
    ang = jnp.arange(seq, dtype=jnp.float32)[:, None] * inv[None, :]
    return jnp.cos(ang), jnp.sin(ang)


def apply_rope(t, cos, sin):
    half = t.shape[-1] // 2
    shape = (1, t.shape[1]) + (1,) * (t.ndim - 3) + (half,)
    c = cos.reshape(shape)
    s = sin.reshape(shape)
    tf = t.astype(jnp.float32)
    t1, t2 = tf[..., :half], tf[..., half:]
    return jnp.concatenate([t1 * c - t2 * s, t2 * c + t1 * s], axis=-1).astype(t.dtype)


def dsa_sparse_attention(q, k, v, q_idx, k_idx, w_idx):
    bsz, seq, nh, dh = q.shape
    topk = min(IDX_TOPK_MAX, seq // 4)
    key_chunk = jnp.arange(seq) // CHUNK
    scale = dh ** -0.5

    def block(i):
        start = i * QBLOCK
        qb = lax.dynamic_slice_in_dim(q, start, QBLOCK, 1)
        qib = lax.dynamic_slice_in_dim(q_idx, start, QBLOCK, 1)
        wib = lax.dynamic_slice_in_dim(w_idx, start, QBLOCK, 1).astype(jnp.float32)
        q_chunk = (start + jnp.arange(QBLOCK)) // CHUNK
        admissible = key_chunk[None, :] <= q_chunk[:, None]
        rel = jax.nn.relu(jnp.einsum('bqhd,bsd->bqhs', qib, k_idx).astype(jnp.float32))
        score = jnp.einsum('bqhs,bqh->bqs', rel, wib)
        score = jnp.where(admissible[None], score, NEG)
        _, idx = lax.top_k(score, topk)
        valid = key_chunk[idx] <= q_chunk[None, :, None]
        k_sel = jax.vmap(lambda kb, ib: kb[ib])(k, idx)
        v_sel = jax.vmap(lambda vb, ib: vb[ib])(v, idx)
        logits = jnp.einsum('bqhd,bqkhd->bhqk', qb, k_sel).astype(jnp.float32) * scale
        logits = jnp.where(valid[:, None], logits, NEG)
        p = jax.nn.softmax(logits, axis=-1).astype(v.dtype)
        return jnp.einsum('bhqk,bqkhd->bqhd', p, v_sel)

    out = lax.map(block, jnp.arange(seq // QBLOCK))
    return jnp.moveaxis(out, 0, 1).reshape(bsz, seq, nh, dh)


def chunked_relpos_attention(q, k, v, rel_bias):
    bsz, seq, nh, dh = q.shape
    pad = B_PAST_CHUNKS * CHUNK
    band = pad + CHUNK
    kp = jnp.pad(k, ((0, 0), (pad, 0), (0, 0), (0, 0)))
    vp = jnp.pad(v, ((0, 0), (pad, 0), (0, 0), (0, 0)))
    kj = jnp.arange(band)
    dist = jnp.arange(CHUNK)[:, None] + pad - kj[None, :]
    bias = rel_bias[:, jnp.clip(dist, -B_REL_CLIP, B_REL_CLIP) + B_REL_CLIP].astype(jnp.float32)
    scale = dh ** -0.5

    def chunk(c):
        start = c * CHUNK
        qc = lax.dynamic_slice_in_dim(q, start, CHUNK, 1)
        kc = lax.dynamic_slice_in_dim(kp, start, band, 1)
        vc = lax.dynamic_slice_in_dim(vp, start, band, 1)
        valid = kj >= pad - start
        logits = jnp.einsum('bqhd,bshd->bhqs', qc, kc).astype(jnp.float32) * scale + bias[None]
        logits = jnp.where(valid, logits, NEG)
        p = jax.nn.softmax(logits, axis=-1).astype(v.dtype)
        return jnp.einsum('bhqs,bshd->bqhd', p, vc)

    out = lax.map(chunk, jnp.arange(seq // CHUNK))
    return jnp.moveaxis(out, 0, 1).reshape(bsz, seq, nh, dh)


def forgetting_attention(q, k, v, log_f):
    bsz, seq, nh, dh = q.shape
    f_cum = jnp.moveaxis(lax.cumsum(log_f, axis=1), 1, 2)
    pos = jnp.arange(seq)
    scale = dh ** -0.5

    def block(i):
        start = i * QBLOCK
        qb = lax.dynamic_slice_in_dim(q, start, QBLOCK, 1)
        fq = lax.dynamic_slice_in_dim(f_cum, start, QBLOCK, 2)
        causal = pos[None, :] <= (start + jnp.arange(QBLOCK))[:, None]
        logits = (jnp.einsum('bqhd,bshd->bhqs', qb, k).astype(jnp.float32) * scale
                  + fq[..., None] - f_cum[:, :, None, :])
        logits = jnp.where(causal, logits, NEG)
        p = jax.nn.softmax(logits, axis=-1).astype(v.dtype)
        return jnp.einsum('bhqs,bshd->bqhd', p, v)

    out = lax.map(block, jnp.arange(seq // QBLOCK))
    return jnp.moveaxis(out, 0, 1).reshape(bsz, seq, nh, dh)


def sink_window_gqa(q, k, v, sinks):
    bsz, seq, nh, dh = q.shape
    nb = seq // QBLOCK
    qb = q.reshape(bsz, nb, QBLOCK, D_KV_HEADS, D_GROUP, dh)
    kb = k.reshape(bsz, nb, QBLOCK, D_KV_HEADS, dh)
    vb = v.reshape(bsz, nb, QBLOCK, D_KV_HEADS, dh)
    kprev = jnp.pad(kb, ((0, 0), (1, 0), (0, 0), (0, 0), (0, 0)))[:, :-1]
    vprev = jnp.pad(vb, ((0, 0), (1, 0), (0, 0), (0, 0), (0, 0)))[:, :-1]
    kband = jnp.concatenate([kprev, kb], axis=2)
    vband = jnp.concatenate([vprev, vb], axis=2)
    sk = jnp.arange(2 * QBLOCK) - QBLOCK
    cdiff = jnp.arange(QBLOCK)[:, None] // CHUNK - sk[None, :] // CHUNK
    band_mask = (cdiff >= 0) & (cdiff <= D_WINDOW_CHUNKS)
    mask = band_mask[None] & ((jnp.arange(nb)[:, None, None] > 0) | (sk >= 0)[None, None, :])
    logits = jnp.einsum('bnqkgd,bnskd->bnkgqs', qb, kband).astype(jnp.float32) * (dh ** -0.5)
    logits = jnp.where(mask[None, :, None, None], logits, NEG)
    sink_col = jnp.broadcast_to(
        sinks.reshape(D_KV_HEADS, D_GROUP).astype(jnp.float32)[None, None, :, :, None, None],
        logits.shape[:-1] + (1,))
    p = jax.nn.softmax(jnp.concatenate([logits, sink_col], axis=-1), axis=-1)[..., :-1]
    out = jnp.einsum('bnkgqs,bnskd->bnqkgd', p.astype(v.dtype), vband)
    return out.reshape(bsz, seq, nh, dh)


def conv_gated_ffn(x, w_up, conv_w, conv_b, w_down):
    h = x @ w_up
    c = h.shape[-1]
    h = lax.conv_general_dilated(
        h, conv_w.reshape(CONV_WIDTH, 1, c).astype(h.dtype), window_strides=(1,),
        padding=[(CONV_WIDTH - 1, 0)], dimension_numbers=('NWC', 'WIO', 'NWC'),
        feature_group_count=c) + conv_b.astype(h.dtype)
    g, u = jnp.split(h, 2, axis=-1)
    return (jax.nn.silu(g) * u) @ w_down


def even_mixer(xn, w_in, w_out, k_ln_g, k_ln_b, rel_bias, rope_h, rope_i):
    bsz, seq, _ = xn.shape
    qa, ka, va, qi, ki, wi, qb, kb, vb = _split(xn @ w_in, EVEN_SPLITS)
    heads = lambda t, n: t.reshape(bsz, seq, n, HEAD_DIM)
    qa = apply_rope(heads(qa, A_HEADS), *rope_h)
    ka = apply_rope(heads(ka, A_HEADS), *rope_h)
    qi = apply_rope(qi.reshape(bsz, seq, IDX_HEADS, IDX_DIM), *rope_i)
    ki = apply_rope(layer_norm(ki, k_ln_g, k_ln_b), *rope_i)
    ya = dsa_sparse_attention(qa, ka, heads(va, A_HEADS), qi, ki, wi)
    yb = chunked_relpos_attention(heads(qb, B_HEADS), heads(kb, B_HEADS), heads(vb, B_HEADS), rel_bias)
    y = jnp.concatenate([ya.reshape(bsz, seq, -1), yb.reshape(bsz, seq, -1)], axis=-1)
    return y @ w_out


def odd_mixer(xn, w_in, w_out, forget_b, sinks, rope_h):
    bsz, seq, _ = xn.shape
    qc, kc, vc, fc, qd, kd, vd = _split(xn @ w_in, ODD_SPLITS)
    heads = lambda t, n: t.reshape(bsz, seq, n, HEAD_DIM)
    log_f = jax.nn.log_sigmoid(fc.astype(jnp.float32) + forget_b.astype(jnp.float32))
    yc = forgetting_attention(heads(qc, C_HEADS), heads(kc, C_HEADS), heads(vc, C_HEADS), log_f)
    yd = sink_window_gqa(apply_rope(heads(qd, D_HEADS), *rope_h),
                         apply_rope(heads(kd, D_KV_HEADS), *rope_h),
                         heads(vd, D_KV_HEADS), sinks)
    y = jnp.concatenate([yc.reshape(bsz, seq, -1), yd.reshape(bsz, seq, -1)], axis=-1)
    return y @ w_out


def setup_inputs(seed: int = 0) -> dict:
    key = jax.random.key(seed)
    ks = jax.random.split(key, 17)
    f32 = jnp.float32
    nrm = lambda k, shape, s: jax.random.normal(k, shape, f32) * s
    return {
        'x': nrm(ks[0], (BATCH, SEQ, D_MODEL), 1.0),
        'norm_mix_g': 1.0 + nrm(ks[1], (DEPTH, D_MODEL), 0.1),
        'norm_ffn_g': 1.0 + nrm(ks[2], (DEPTH, D_MODEL), 0.1),
        'norm_out_g': 1.0 + nrm(ks[3], (D_MODEL,), 0.1),
        'even_w_in': nrm(ks[4], (N_EVEN, D_MODEL, EVEN_IN), D_MODEL ** -0.5),
        'even_w_out': nrm(ks[5], (N_EVEN, EVEN_OUT, D_MODEL), EVEN_OUT ** -0.5),
        'idx_k_ln_g': 1.0 + nrm(ks[6], (N_EVEN, IDX_DIM), 0.1),
        'idx_k_ln_b': nrm(ks[7], (N_EVEN, IDX_DIM), 0.02),
        'rel_bias': nrm(ks[8], (N_EVEN, B_HEADS, 2 * B_REL_CLIP + 1), 0.5),
        'odd_w_in': nrm(ks[9], (N_ODD, D_MODEL, ODD_IN), D_MODEL ** -0.5),
        'odd_w_out': nrm(ks[10], (N_ODD, ODD_OUT, D_MODEL), ODD_OUT ** -0.5),
        'forget_b': jax.random.uniform(ks[11], (N_ODD, C_HEADS), f32, 1.0, 5.0),
        'sinks': nrm(ks[12], (N_ODD, D_HEADS), 1.0),
        'ffn_w_up': nrm(ks[13], (DEPTH, D_MODEL, 2 * D_FF), D_MODEL ** -0.5),
        'ffn_conv_w': nrm(ks[14], (DEPTH, CONV_WIDTH, 2 * D_FF), CONV_WIDTH ** -0.5),
        'ffn_conv_b': nrm(ks[15], (DEPTH, 2 * D_FF), 0.02),
        'ffn_w_down': nrm(ks[16], (DEPTH, D_FF, D_MODEL), D_FF ** -0.5),
    }


def reference(x, norm_mix_g, norm_ffn_g, norm_out_g, even_w_in, even_w_out, idx_k_ln_g, idx_k_ln_b,
              rel_bias, odd_w_in, odd_w_out, forget_b, sinks, ffn_w_up, ffn_conv_w, ffn_conv_b, ffn_w_down):
    seq = x.shape[1]
    rope_h = rope_tables(seq, HEAD_DIM)
    rope_i = rope_tables(seq, IDX_DIM)
    h = x
    for layer in range(DEPTH):
        j = layer // 2
        xn = rms_norm(h, norm_mix_g[layer])
        if layer % 2 == 0:
            mix = even_mixer(xn, even_w_in[j], even_w_out[j], idx_k_ln_g[j], idx_k_ln_b[j],
                             rel_bias[j], rope_h, rope_i)
        else:
            mix = odd_mixer(xn, odd_w_in[j], odd_w_out[j], forget_b[j], sinks[j], rope_h)
        h = h + mix
        h = h + conv_gated_ffn(rms_norm(h, norm_ffn_g[layer]), ffn_w_up[layer], ffn_conv_w[layer],
                               ffn_conv_b[layer], ffn_w_down[layer])
    return rms_norm(h, norm_out_g)
```

```python
import numpy as np
import ml_dtypes
from contextlib import ExitStack


import concourse.bass as bass
import concourse.mybir as mybir
from concourse.bass_utils import run_bass_kernel_spmd

F32 = mybir.dt.float32
BF16 = mybir.dt.bfloat16
AF = mybir.ActivationFunctionType
ALU = mybir.AluOpType
AX = mybir.AxisListType

ENGS = ("pe", "act", "dve", "pool", "sp")


class Res:
    __slots__ = ("name", "w", "readers", "dsem", "dcnt")

    def __init__(self, name):
        self.name = name
        self.w = None
        self.readers = []
        self.dsem = None
        self.dcnt = 0


class Op:
    __slots__ = ("eng", "fn", "deps", "sig", "seq", "dma", "tok")

    def __init__(self, eng, fn):
        self.eng = eng
        self.fn = fn
        self.deps = []
        self.sig = False
        self.seq = 0
        self.dma = False
        self.tok = None


class Prog:
    def __init__(self, sync_same_engine=True):
        self.nc = bass.Bass("TRN2", target_bir_lowering=False)
        self.ops = []
        self.stack = None
        self.sync_same = sync_same_engine
        self.dma_res = []
        self._n = 0

    def sb(self, shape, dtype, name=None):
        self._n += 1
        t = self.stack.enter_context(self.nc.sbuf_tensor(name or f"sb{self._n}", list(shape), dtype))
        return t

    def ps(self, shape, dtype, name=None):
        self._n += 1
        t = self.stack.enter_context(self.nc.psum_tensor(name or f"ps{self._n}", list(shape), dtype))
        return t

    def res(self, name=None):
        self._n += 1
        return Res(name or f"r{self._n}")

    def dram_in(self, name, shape, dtype):
        return self.nc.dram_tensor(name, list(shape), dtype, kind="ExternalInput").ap()

    def dram_out(self, name, shape, dtype):
        return self.nc.dram_tensor(name, list(shape), dtype, kind="ExternalOutput").ap()

    def _track(self, op, reads, writes):
        deps = op.deps
        for r in reads:
            if r.w is not None:
                deps.append(r.w)
        for w in writes:
            if w.w is not None:
                deps.append(w.w)
            deps.extend(w.readers)
        for r in reads:
            r.readers.append(op)
        for w in writes:
            w.w = op
            w.readers = []
        self.ops.append(op)

    def op(self, eng, fn, reads=(), writes=()):
        o = Op(eng, fn)
        self._track(o, reads, writes)
        return o

    def dma(self, queue, out, in_, reads=(), writes=(), sem_res=None, **kw):
        if sem_res is None:
            sem_res = (list(writes) + list(reads))[0]
        if sem_res.dsem is None:
            self._n += 1
            sem_res.dsem = self.stack.enter_context(self.nc.semaphore(f"dq{self._n}"))
            self.dma_res.append(sem_res)
        sem_res.dcnt += 1
        o = Op(queue, lambda e: e.dma_start(out=out, in_=in_, **kw))
        o.dma = True
        o.tok = (sem_res.dsem, 16 * sem_res.dcnt)
        self._track(o, reads, writes)
        return o

    def emit(self, final_wait_eng="sp"):
        nc = self.nc
        sems = {e: self.stack.enter_context(nc.semaphore(f"eng_{e}")) for e in ENGS}
        for i, o in enumerate(self.ops):
            o.seq = i
        per_eng = {e: [] for e in ENGS}
        waited_idx = {e: {} for e in ENGS}
        waited_dma = {e: {} for e in ENGS}
        nwaits = 0
        for o in self.ops:
            wl_eng = {}
            wl_dma = {}
            for d in o.deps:
                if d.dma:
                    sem, val = d.tok
                    key = id(sem)
                    if waited_dma[o.eng].get(key, 0) >= val:
                        continue
                    if key not in wl_dma or wl_dma[key][1] < val:
                        wl_dma[key] = (sem, val)
                else:
                    if d.eng == o.eng and (d.eng == "pe" or not self.sync_same):
                        continue
                    if waited_idx[o.eng].get(d.eng, -1) >= d.seq:
                        continue
                    if d.eng not in wl_eng or wl_eng[d.eng].seq < d.seq:
                        wl_eng[d.eng] = d
            for key, (sem, val) in wl_dma.items():
                waited_dma[o.eng][key] = val
            for de, d in wl_eng.items():
                waited_idx[o.eng][de] = d.seq
                d.sig = True
            nwaits += len(wl_dma) + len(wl_eng)
            per_eng[o.eng].append((o, list(wl_dma.values()), list(wl_eng.values())))
        cnt = {e: 0 for e in ENGS}
        for o in self.ops:
            if o.dma:
                continue
            if o.sig:
                cnt[o.eng] += 1
                o.seq = cnt[o.eng]
        assert max(cnt.values()) < 60000, cnt
        finals = [(r.dsem, 16 * r.dcnt) for r in self.dma_res]
        self.stats = dict(n_ops=len(self.ops), n_waits=nwaits, sig={e: cnt[e] for e in ENGS},
                          per_eng={e: len(per_eng[e]) for e in ENGS})

        def run(eng_name, eng):
            for o, wl, wd in per_eng[eng_name]:
                for sem, val in wl:
                    eng.wait_ge(sem, val)
                for d in wd:
                    eng.wait_ge(sems[d.eng], d.seq)
                ins = o.fn(eng)
                if o.dma:
                    ins.then_inc(o.tok[0], 16)
                elif o.sig:
                    ins.then_inc(sems[eng_name], 1)
            if eng_name == final_wait_eng:
                for sem, val in finals:
                    eng.wait_ge(sem, val)

        with nc.Block() as block:
            @block.tensor
            def _(e):
                run("pe", e)

            @block.scalar
            def _(e):
                run("act", e)

            @block.vector
            def _(e):
                run("dve", e)

            @block.gpsimd
            def _(e):
                run("pool", e)

            @block.sync
            def _(e):
                run("sp", e)


EPS = 1e-6
D = 1024
T = 4096


def build_proj(layer):
    P = Prog()
    nc = P.nc
    if layer == 0:
        C = 3396
        CB = 3392
        groups = [(0, 512), (512, 1024), (1024, 1536), (1536, 1860), (1860, 2372), (2372, 2884), (2884, 3396)]
        NF = 4
    else:
        C = 2312
        CB = 2304
        groups = [(0, 512), (512, 1024), (1024, 1536), (1536, 2048), (2048, 2312)]
        NF = 8
    hT = P.dram_in("hT", [D, T], F32)
    g_in = P.dram_in("g", [128, 8], F32)
    W_in = P.dram_in("W", [128, 8, C], F32)
    cs_in = P.dram_in("cs", [T, 128], F32)
    ex_in = P.dram_in("ex", [128, 128], F32)
    pb = P.dram_out("pb", [T, CB], BF16)
    pf = P.dram_out("pf", [T, NF], F32)
    hTv = hT.rearrange("(k p) t -> p k t", p=128)

    with ExitStack() as st:
        P.stack = st
        Wbf = P.sb([128, 8, C], BF16); rW = [P.res() for _ in range(8)]
        stage = [P.sb([128, C], F32) for _ in range(2)]; rstage = [P.res() for _ in range(2)]
        g_sb = P.sb([128, 8], F32); rg = P.res()
        ex_sb = P.sb([128, 128], F32); rex = P.res()
        ones = P.sb([128, 1], F32); rones = P.res()
        P.dma("sp", g_sb[:], g_in[:, :], writes=[rg])
        P.dma("sp", ex_sb[:], ex_in[:, :], writes=[rex])
        P.op("dve", lambda e: e.memset(ones[:], 1.0), writes=[rones])
        for k in range(8):
            s = k % 2
            P.dma("sp", stage[s][:], W_in[:, k, :], writes=[rstage[s]])
            eng = "dve" if k % 2 == 0 else "pool"
            P.op(eng, lambda e, k=k, s=s: e.tensor_scalar(out=Wbf[:, k, :], in0=stage[s][:], scalar1=g_sb[:, k:k + 1],
                                                          scalar2=None, op0=ALU.mult),
                 reads=[rstage[s], rg], writes=[rW[k]])

        NB = 2
        hblk = [P.sb([128, 8, 256], F32) for _ in range(NB)]; rh = [P.res() for _ in range(NB)]
        sq = [P.sb([128, 8, 256], F32) for _ in range(NB)]; rsq = [P.res() for _ in range(NB)]
        xb = [P.sb([128, 8, 256], BF16) for _ in range(NB)]; rxb = [P.res() for _ in range(NB)]
        ss_ps = P.ps([128, 16], F32); rss = P.res()
        NPS = 3
        pps = [P.ps([128, 512], F32) for _ in range(NPS)]; rpps = [P.res() for _ in range(NPS)]
        NT = 2
        tmp = [P.sb([128, 1860 if layer == 0 else 1024], F32) for _ in range(NT)]
        rtmp = [P.res() for _ in range(NT)]
        obf = [P.sb([128, CB], BF16) for _ in range(NT)]; robf = [P.res() for _ in range(NT)]
        off = [P.sb([128, NF], F32) for _ in range(NT)]; roff = [P.res() for _ in range(NT)]
        cst = [P.sb([128, 128], F32) for _ in range(NT)]; rcs = [P.res() for _ in range(NT)]
        st_s = [P.sb([128, 4], F32) for _ in range(NT)]; rst = [P.res() for _ in range(NT)]
        ra = [P.sb([128, 8, 32], F32) for _ in range(2)]; rra = [P.res() for _ in range(2)]
        lnst = P.sb([128, 8], F32); rln = P.res()
        lnx = P.sb([128, 64], F32); rlnx = P.res()
        ipsum = 0

        def rope(src, H, dst, c_ap, s_ap, rsrc, rdst, rc):
            sv = src.rearrange("p (h two j) -> p h two j", h=H, two=2)
            dv = dst.rearrange("p (h two j) -> p h two j", h=H, two=2)
            x1, x2 = sv[:, :, 0, :], sv[:, :, 1, :]
            cb = c_ap.unsqueeze(1).to_broadcast([128, H, 32])
            sbb = s_ap.unsqueeze(1).to_broadcast([128, H, 32])
            a, b = ra[0][:, 0:H, :], ra[1][:, 0:H, :]
            P.op("dve", lambda e: e.tensor_tensor(out=a, in0=x1, in1=cb, op=ALU.mult), reads=[rsrc, rc], writes=[rra[0]])
            P.op("dve", lambda e: e.tensor_tensor(out=b, in0=x2, in1=sbb, op=ALU.mult), reads=[rsrc, rc], writes=[rra[1]])
            P.op("dve", lambda e: e.tensor_tensor(out=dv[:, :, 0, :], in0=a, in1=b, op=ALU.subtract),
                 reads=[rra[0], rra[1]], writes=[rdst])
            P.op("dve", lambda e: e.tensor_tensor(out=a, in0=x2, in1=cb, op=ALU.mult), reads=[rsrc, rc], writes=[rra[0]])
            P.op("dve", lambda e: e.tensor_tensor(out=b, in0=x1, in1=sbb, op=ALU.mult), reads=[rsrc, rc], writes=[rra[1]])
            P.op("dve", lambda e: e.tensor_tensor(out=dv[:, :, 1, :], in0=a, in1=b, op=ALU.add),
                 reads=[rra[0], rra[1]], writes=[rdst])

        for blk in range(T // 256):
            bs = blk % NB
            P.dma("sp", hblk[bs][:], hTv[:, :, blk * 256:(blk + 1) * 256], writes=[rh[bs]])
            P.op("pool", lambda e, bs=bs: e.tensor_tensor(out=sq[bs][:], in0=hblk[bs][:], in1=hblk[bs][:], op=ALU.mult),
                 reads=[rh[bs]], writes=[rsq[bs]])
            P.op("dve", lambda e, bs=bs: e.tensor_copy(out=xb[bs][:], in_=hblk[bs][:]), reads=[rh[bs]], writes=[rxb[bs]])
            for tt in range(2):
                ti = blk * 2 + tt
                ts_ = ti % NT
                tok = slice(tt * 128, (tt + 1) * 128)
                P.dma("sp", cst[ts_][:], cs_in[ti * 128:(ti + 1) * 128, :], writes=[rcs[ts_]])
                for k in range(8):
                    P.op("pe", lambda e, k=k, bs=bs, tok=tok: e.matmul(ss_ps[:, 0:1], lhsT=sq[bs][:, k, tok], rhs=ones[:],
                                                                      start=(k == 0), stop=(k == 7)),
                         reads=[rsq[bs], rones], writes=[rss])
                stt = st_s[ts_]
                P.op("act", lambda e, stt=stt: e.activation(out=stt[:, 0:1], in_=ss_ps[:, 0:1], func=AF.Sqrt,
                                                            bias=EPS, scale=1.0 / D),
                     reads=[rss], writes=[rst[ts_]])
                P.op("dve", lambda e, stt=stt: e.reciprocal(out=stt[:, 1:2], in_=stt[:, 0:1]), reads=[rst[ts_]], writes=[rst[ts_]])
                P.op("dve", lambda e, stt=stt: e.tensor_scalar(out=stt[:, 2:3], in0=stt[:, 1:2], scalar1=0.125, scalar2=None,
                                                               op0=ALU.mult), reads=[rst[ts_]], writes=[rst[ts_]])
                tm, ob, of = tmp[ts_], obf[ts_], off[ts_]
                if layer == 0:
                    plan = [(tm[:, 0:512], 1), (tm[:, 512:1024], 1), (ob[:, 1024:1536], 1), (tm[:, 1536:1860], 1),
                            (ob[:, 1856:2368], 2), (ob[:, 2368:2880], 1), (ob[:, 2880:3392], 1)]
                    gw = [(True, False), (True, False), (False, True), (True, False), (False, True), (False, True), (False, True)]
                else:
                    plan = [(ob[:, 0:512], 2), (ob[:, 512:1024], 1), (ob[:, 1024:1536], 1), (tm[:, 0:512], 1),
                            (tm[:, 512:776], 1)]
                    gw = [(False, True), (False, True), (False, True), (True, False), (True, False)]
                for gi, (c0, c1) in enumerate(groups):
                    pi = ipsum % NPS
                    ipsum += 1
                    w_ = c1 - c0
                    for k in range(8):
                        P.op("pe", lambda e, k=k, bs=bs, tok=tok, pi=pi, c0=c0, c1=c1, w_=w_: e.matmul(
                            pps[pi][:, 0:w_], lhsT=xb[bs][:, k, tok], rhs=Wbf[:, k, c0:c1], start=(k == 0), stop=(k == 7)),
                            reads=[rxb[bs], rW[k]], writes=[rpps[pi]])
                    dst, sc = plan[gi]
                    wr = []
                    if gw[gi][0]:
                        wr.append(rtmp[ts_])
                    if gw[gi][1]:
                        wr.append(robf[ts_])
                    P.op("act", lambda e, dst=dst, pi=pi, w_=w_, sc=sc, stt=stt: e.activation(
                        out=dst, in_=pps[pi][:, 0:w_], func=AF.Copy, scale=stt[:, sc:sc + 1]),
                        reads=[rpps[pi], rst[ts_]], writes=wr)
                c_, s_, c8, s8 = cst[ts_][:, 0:32], cst[ts_][:, 32:64], cst[ts_][:, 64:96], cst[ts_][:, 96:128]
                if layer == 0:
                    rope(tm[:, 0:512], 8, ob[:, 0:512], c8, s8, rtmp[ts_], robf[ts_], rcs[ts_])
                    rope(tm[:, 512:1024], 8, ob[:, 512:1024], c_, s_, rtmp[ts_], robf[ts_], rcs[ts_])
                    rope(tm[:, 1536:1792], 4, ob[:, 1536:1792], c_, s_, rtmp[ts_], robf[ts_], rcs[ts_])
                    ki = tm[:, 1792:1856]
                    P.op("dve", lambda e, ki=ki: e.bn_stats(out=lnst[:, 0:6], in_=ki), reads=[rtmp[ts_]], writes=[rln])
                    P.op("dve", lambda e: e.bn_aggr(out=lnst[:, 6:8], in_=lnst[:, 0:6]), reads=[rln], writes=[rln])
                    P.op("act", lambda e: e.activation(out=lnst[:, 0:1], in_=lnst[:, 7:8], func=AF.Sqrt, bias=EPS, scale=1.0),
                         reads=[rln], writes=[rln])
                    P.op("dve", lambda e: e.reciprocal(out=lnst[:, 1:2], in_=lnst[:, 0:1]), reads=[rln], writes=[rln])
                    P.op("dve", lambda e, ki=ki: e.tensor_scalar(out=lnx[:], in0=ki, scalar1=lnst[:, 6:7], scalar2=lnst[:, 1:2],
                                                                 op0=ALU.subtract, op1=ALU.mult),
                         reads=[rtmp[ts_], rln], writes=[rlnx])
                    P.op("dve", lambda e: e.tensor_tensor(out=lnx[:], in0=lnx[:], in1=ex_sb[:, 0:64], op=ALU.mult),
                         reads=[rlnx, rex], writes=[rlnx])
                    P.op("dve", lambda e: e.tensor_tensor(out=lnx[:], in0=lnx[:], in1=ex_sb[:, 64:128], op=ALU.add),
                         reads=[rlnx, rex], writes=[rlnx])
                    rope(lnx[:], 1, ob[:, 1792:1856], c_, s_, rlnx, robf[ts_], rcs[ts_])
                    P.op("pool", lambda e, of=of, tm=tm: e.tensor_copy(out=of[:, 0:4], in_=tm[:, 1856:1860]),
                         reads=[rtmp[ts_]], writes=[roff[ts_]])
                else:
                    rope(tm[:, 0:512], 8, ob[:, 1536:2048], c8, s8, rtmp[ts_], robf[ts_], rcs[ts_])
                    rope(tm[:, 512:640], 2, ob[:, 2048:2176], c_, s_, rtmp[ts_], robf[ts_], rcs[ts_])
                    P.op("pool", lambda e, ob=ob, tm=tm: e.tensor_copy(out=ob[:, 2176:2304], in_=tm[:, 640:768]),
                         reads=[rtmp[ts_]], writes=[robf[ts_]])
                    P.op("dve", lambda e, tm=tm: e.tensor_tensor(out=lnx[:, 0:8], in0=tm[:, 768:776], in1=ex_sb[:, 0:8], op=ALU.add),
                         reads=[rtmp[ts_], rex], writes=[rlnx])
                    P.op("act", lambda e: e.activation(out=lnx[:, 8:16], in_=lnx[:, 0:8], func=AF.Exp, scale=-1.0),
                         reads=[rlnx], writes=[rlnx])
                    P.op("act", lambda e: e.activation(out=lnx[:, 16:24], in_=lnx[:, 8:16], func=AF.Ln, bias=1.0, scale=1.0),
                         reads=[rlnx], writes=[rlnx])
                    P.op("dve", lambda e, of=of: e.tensor_scalar(out=of[:, 0:8], in0=lnx[:, 16:24], scalar1=-1.0, scalar2=None,
                                                                 op0=ALU.mult), reads=[rlnx], writes=[roff[ts_]])
                P.dma("sp", pb[ti * 128:(ti + 1) * 128, :], ob[:], reads=[robf[ts_]])
                P.dma("sp", pf[ti * 128:(ti + 1) * 128, :], of[:], reads=[roff[ts_]])
        P.emit()
    return P


S = 16384
NM = 32


class AttnCtx:
    def __init__(self, P):
        self.P = P
        self.Sps = [P.ps([128, 1024], F32) for _ in range(2)]
        self.rS = [P.res() for _ in range(2)]
        self.Ops = P.ps([128, 1024], F32)
        self.rO = P.res()
        NPX = 3
        self.Pexp = [P.sb([128, 1024], BF16) for _ in range(NPX)]
        self.rPexp = [P.res() for _ in range(NPX)]
        self.PT = [P.sb([128, 1024], BF16) for _ in range(NPX)]
        self.rPT = [P.res() for _ in range(NPX)]
        self.ysb = [P.sb([128, 512], BF16) for _ in range(2)]
        self.rysb = [P.res() for _ in range(2)]
        self.den = P.sb([128, 16], F32)
        self.rden = P.res()
        self.iu = 0
        self.iy = 0
        self.pending = None

    def ocol(self, h):
        return (h // 4) * 512 + (h % 4) * 65

    def qk(self, u):
        P = self.P
        s = u["s"]
        Kd = u["Kd"]
        for h in range(8):
            P.op("pe", lambda e, h=h, s=s, u=u: e.matmul(self.Sps[s][:, h * 128:(h + 1) * 128], lhsT=u["kt"](h), rhs=u["qt"](h),
                                                          start=True, stop=True),
                 reads=u["rk"] + u["rq"], writes=[self.rS[s]])

    def post(self, u):
        P = self.P
        s = u["s"]
        px = u["px"]
        if u.get("clamp"):
            P.op("dve", lambda e, s=s: e.tensor_scalar(out=self.Sps[s][:], in0=self.Sps[s][:], scalar1=60.0, scalar2=None, op0=ALU.min),
                 reads=[self.rS[s]], writes=[self.rS[s]])
        for half in range(2):
            P.op("act", lambda e, half=half, s=s, px=px: e.activation(out=self.Pexp[px][:, half * 512:(half + 1) * 512],
                                                                      in_=self.Sps[s][:, half * 512:(half + 1) * 512], func=AF.Exp),
                 reads=[self.rS[s]], writes=[self.rPexp[px]])
        if u["E"] is not None:
            Eap = u["E"]
            P.op("dve", lambda e, px=px, Eap=Eap: e.tensor_tensor(out=self.PT[px][:].rearrange("p (h q) -> p h q", h=8),
                                                                  in0=self.Pexp[px][:].rearrange("p (h q) -> p h q", h=8),
                                                                  in1=Eap, op=ALU.mult),
                 reads=[self.rPexp[px]] + u["rE"], writes=[self.rPT[px]])
            src, rsrc = self.PT[px], self.rPT[px]
        else:
            src, rsrc = self.Pexp[px], self.rPexp[px]
        for h in range(8):
            oc = self.ocol(h)
            P.op("pe", lambda e, h=h, oc=oc, src=src, u=u: e.matmul(self.Ops[:, oc:oc + 65], lhsT=src[:, h * 128:(h + 1) * 128],
                                                                    rhs=u["v"](h), start=(u["first"] and h % 4 == 0),
                                                                    stop=u["last"], skip_group_check=True),
                 reads=[rsrc] + u["rv"], writes=[self.rO])
        if u["last"]:
            self.epilogue(u)

    def epilogue(self, u):
        P = self.P
        den = self.den
        for b in range(2):
            Ov = self.Ops[:, b * 512:b * 512 + 260].rearrange("p (h e) -> p h e", h=4)
            P.op("dve", lambda e, b=b, Ov=Ov: e.tensor_copy(out=den[:, b * 4:(b + 1) * 4], in_=Ov[:, :, 64]),
                 reads=[self.rO], writes=[self.rden])
        if u.get("sink") is not None:
            sk, rsk = u["sink"]
            P.op("dve", lambda e, sk=sk: e.tensor_tensor(out=den[:, 0:8], in0=den[:, 0:8], in1=sk, op=ALU.add),
                 reads=[self.rden, rsk], writes=[self.rden])
        P.op("dve", lambda e: e.tensor_scalar(out=den[:, 0:8], in0=den[:, 0:8], scalar1=1e-30, scalar2=None, op0=ALU.max),
             reads=[self.rden], writes=[self.rden])
        P.op("dve", lambda e: e.reciprocal(out=den[:, 8:16], in_=den[:, 0:8]), reads=[self.rden], writes=[self.rden])
        iy = self.iy % 2
        self.iy += 1
        ysb = self.ysb[iy]
        for b in range(2):
            Ov = self.Ops[:, b * 512:b * 512 + 260].rearrange("p (h e) -> p h e", h=4)
            P.op("dve", lambda e, b=b, Ov=Ov, ysb=ysb: e.tensor_tensor(
                out=ysb[:, b * 256:(b + 1) * 256].rearrange("p (h e) -> p h e", h=4), in0=Ov[:, :, 0:64],
                in1=den[:, 8 + b * 4:8 + (b + 1) * 4].unsqueeze(2).to_broadcast([128, 4, 64]), op=ALU.mult),
                reads=[self.rO, self.rden], writes=[self.rysb[iy]])
        P.dma("sp", u["ydst"], ysb[:], reads=[self.rysb[iy]])

    def unit(self, u):
        u["s"] = self.iu % 2
        u["px"] = self.iu % 3
        self.iu += 1
        self.qk(u)
        if self.pending is not None:
            self.post(self.pending)
        self.pending = u

    def flush(self):
        if self.pending is not None:
            self.post(self.pending)
            self.pending = None


def build_attn1():
    P = Prog()
    QTc = P.dram_in("QTc", [NM, 70, 1024], BF16)
    KTc = P.dram_in("KTc", [8, 70, S], BF16)
    Vc = P.dram_in("Vc", [S // 128, 128, 520], BF16)
    mC = P.dram_in("mC", [128, 512], BF16)
    QTd = P.dram_in("QTd", [NM, 64, 1024], BF16)
    KTd = P.dram_in("KTd", [NM, 64, 512], BF16)
    Vd = P.dram_in("Vd", [NM, 128, 260], BF16)
    mD = P.dram_in("mD", [2, 128, 256], BF16)
    snk = P.dram_in("snk", [128, 8], F32)
    y = P.dram_out("y", [NM, 128, 1024], BF16)
    with ExitStack() as st:
        P.stack = st
        A = AttnCtx(P)
        mC_s = P.sb([128, 512], BF16); rmC = P.res()
        mD_s = P.sb([128, 512], BF16); rmD = P.res()
        sk_s = P.sb([128, 8], F32); rsk = P.res()
        P.dma("sp", mC_s[:], mC[:, :], writes=[rmC])
        P.dma("sp", mD_s[:].rearrange("p (v c) -> p v c", v=2), mD.rearrange("v p c -> p v c"), writes=[rmD])
        P.dma("sp", sk_s[:], snk[:, :], writes=[rsk])
        P.op("act", lambda e: e.activation(out=sk_s[:], in_=sk_s[:], func=AF.Exp), reads=[rsk], writes=[rsk])
        NK = 3
        KTs = [P.sb([70, 8, 512], BF16) for _ in range(NK)]; rKT = [P.res() for _ in range(NK)]
        Vs = [P.sb([128, 4, 520], BF16) for _ in range(NK)]; rV = [P.res() for _ in range(NK)]
        QTs = [P.sb([70, 1024], BF16) for _ in range(2)]; rQT = [P.res() for _ in range(2)]
        QDs = [P.sb([64, 1024], BF16) for _ in range(2)]; rQD = [P.res() for _ in range(2)]
        KDs = [P.sb([64, 512], BF16) for _ in range(2)]; rKD = [P.res() for _ in range(2)]
        VDs = [P.sb([128, 260], BF16) for _ in range(2)]; rVD = [P.res() for _ in range(2)]
        ig = 0
        for m in range(NM):
            qs = m % 2
            P.dma("sp", QDs[qs][:], QTd[m, :, :], writes=[rQD[qs]])
            P.dma("sp", KDs[qs][:], KTd[m, :, :], writes=[rKD[qs]])
            P.dma("sp", VDs[qs][:], Vd[m, :, :], writes=[rVD[qs]])
            var = 0 if m == 0 else 1
            for blk in range(2):
                A.unit(dict(
                    Kd=64,
                    kt=lambda h, qs=qs, blk=blk: KDs[qs][:, (h // 4) * 256 + blk * 128:(h // 4) * 256 + (blk + 1) * 128],
                    qt=lambda h, qs=qs: QDs[qs][:, h * 128:(h + 1) * 128],
                    v=lambda h, qs=qs, blk=blk: VDs[qs][:, blk * 130 + (h // 4) * 65:blk * 130 + (h // 4) * 65 + 65],
                    rk=[rKD[qs]], rq=[rQD[qs]], rv=[rVD[qs]],
                    E=mD_s[:, var * 256 + blk * 128:var * 256 + (blk + 1) * 128].unsqueeze(1).to_broadcast([128, 8, 128]),
                    rE=[rmD], first=(blk == 0), last=(blk == 1), ydst=y[m, :, 512:1024], sink=(sk_s[:, 0:8], rsk)))
            P.dma("sp", QTs[qs][:], QTc[m, :, :], writes=[rQT[qs]])
            for g in range(m + 1):
                ks = ig % NK
                ig += 1
                P.dma("sp", KTs[ks][:], KTc[:, :, g * 512:(g + 1) * 512].rearrange("h d s -> d h s"), writes=[rKT[ks]])
                P.dma("sp", Vs[ks][:], Vc[g * 4:(g + 1) * 4, :, :].rearrange("b p e -> p b e"), writes=[rV[ks]])
                for blk in range(4):
                    diag = (g == m)
                    A.unit(dict(
                        Kd=70,
                        kt=lambda h, ks=ks, blk=blk: KTs[ks][:, h, blk * 128:(blk + 1) * 128],
                        qt=lambda h, qs=qs: QTs[qs][:, h * 128:(h + 1) * 128],
                        v=lambda h, ks=ks, blk=blk: Vs[ks][:, blk, h * 65:(h + 1) * 65],
                        rk=[rKT[ks]], rq=[rQT[qs]], rv=[rV[ks]],
                        E=(mC_s[:, blk * 128:(blk + 1) * 128].unsqueeze(1).to_broadcast([128, 8, 128]) if diag else None),
                        rE=[rmC], first=(g == 0 and blk == 0), last=(diag and blk == 3), ydst=y[m, :, 0:512], clamp=diag))
        A.flush()
        P.emit()
    return P


def build_fcum():
    P = Prog()
    lf = P.dram_in("lf", [8, S], F32)
    fo = P.dram_out("fo", [8, 6, S], BF16)
    CH = 4096
    with ExitStack() as st:
        P.stack = st
        lfs = P.sb([8, CH], F32); rl = P.res()
        one = P.sb([8, CH], F32); r1 = P.res()
        F = P.sb([8, CH], F32); rF = P.res()
        t32 = P.sb([8, CH], F32); rt = P.res()
        carry = P.sb([8, 1], F32); rc = P.res()
        pc = [P.sb([8, CH], BF16) for _ in range(6)]; rp = [P.res() for _ in range(6)]
        P.op("pool", lambda e: e.memset(one[:], 1.0), writes=[r1])
        P.op("pool", lambda e: e.memset(carry[:], 0.0), writes=[rc])
        for c in range(S // CH):
            sl = slice(c * CH, (c + 1) * CH)
            P.dma("sp", lfs[:], lf[:, sl], writes=[rl])
            P.op("dve", lambda e: e.tensor_tensor_scan(out=F[:], data0=one[:], data1=lfs[:], initial=carry[:, 0:1],
                                                       op0=ALU.mult, op1=ALU.add),
                 reads=[rl, r1, rc], writes=[rF])
            P.op("dve", lambda e: e.tensor_copy(out=carry[:], in_=F[:, CH - 1:CH]), reads=[rF], writes=[rc])
            for i in range(3):
                P.op("dve", lambda e, i=i: e.tensor_copy(out=pc[i][:], in_=F[:]), reads=[rF], writes=[rp[i]])
                P.op("dve", lambda e, i=i: e.tensor_scalar(out=pc[3 + i][:], in0=pc[i][:], scalar1=-1.0, scalar2=None, op0=ALU.mult),
                     reads=[rp[i]], writes=[rp[3 + i]])
                if i < 2:
                    P.op("dve", lambda e, i=i: e.tensor_copy(out=t32[:], in_=pc[i][:]), reads=[rp[i]], writes=[rt])
                    P.op("dve", lambda e: e.tensor_tensor(out=F[:], in0=F[:], in1=t32[:], op=ALU.subtract), reads=[rF, rt], writes=[rF])
            for i in range(6):
                P.dma("sp", fo[:, i, sl], pc[i][:], reads=[rp[i]])
        P.emit()
    return P


def build_attn0(nm=NM, debug=False, dbg_tile=None):
    P = Prog()
    QTa = P.dram_in("QTa", [NM, 64, 1024], BF16)
    KTa = P.dram_in("KTa", [8, 64, S], BF16)
    Va = P.dram_in("Va", [S // 128, 128, 520], BF16)
    qiT = P.dram_in("qiT", [NM, 64, 512], BF16)
    kiT = P.dram_in("kiT", [64, S], BF16)
    wi = P.dram_in("wi", [NM, 128, 4], F32)
    mAneg = P.dram_in("mAneg", [128, 512], F32)
    ident = P.dram_in("ident", [128, 128], BF16)
    QTb = P.dram_in("QTb", [NM, 64, 1024], BF16)
    KTb = P.dram_in("KTb", [NM, 64, 5120], BF16)
    Vb = P.dram_in("Vb", [NM, 128, 2600], BF16)
    bB = P.dram_in("bB", [128, 5120], F32)
    mB = P.dram_in("mB", [2, 128, 640], BF16)
    y = P.dram_out("y", [NM, 128, 1024], BF16)
    NIT = 16
    CHK = 1024
    F16 = mybir.dt.float16
    if dbg_tile is None:
        dbg_tile = nm - 1
    if debug:
        d_sc = P.dram_out('d_sc', [128, 512 * (dbg_tile + 1)], F32)
        d_mk = P.dram_out('d_mk', [128, 512 * (dbg_tile + 1)], BF16)
        d_bs = P.dram_out('d_bs', [128, 8 + NIT + 1], F32)
    with ExitStack() as st:
        P.stack = st
        A = AttnCtx(P)
        Tps = [P.ps([128, 1024], BF16) for _ in range(2)]; rT = [P.res() for _ in range(2)]
        sc = P.sb([128, S], F32); rsc = P.res()
        msk = P.sb([128, S], BF16); rmsk = P.res()
        mA_s = P.sb([128, 512], F32); rmA = P.res()
        id_s = P.sb([128, 128], BF16); rid = P.res()
        mB_s = P.sb([128, 2, 640], BF16); rmB = P.res()
        EB = [P.sb([128, 5120], BF16) for _ in range(2)]; rEB = [P.res() for _ in range(2)]
        bst = P.sb([128, 1024], F32); rbst = P.res()
        ctab = P.sb([128, NIT + 1], F32); rct = P.res()
        P.dma("sp", mA_s[:], mAneg[:, :], writes=[rmA])
        P.dma("sp", id_s[:], ident[:, :], writes=[rid])
        P.dma("sp", mB_s[:], mB.rearrange("v p c -> p v c"), writes=[rmB])
        for k in range(NIT + 1):
            P.op("pool", lambda e, k=k: e.memset(ctab[:, k:k + 1], 2.0 ** -(k + 1)), writes=[rct])
        for r in range(5):
            P.dma("sp", bst[:], bB[:, r * 1024:(r + 1) * 1024], writes=[rbst])
            P.op("act", lambda e: e.activation(out=bst[:], in_=bst[:], func=AF.Exp), reads=[rbst], writes=[rbst])
            for v in range(2):
                P.op("dve", lambda e, r=r, v=v: e.tensor_tensor(
                    out=EB[v][:, r * 1024:(r + 1) * 1024].rearrange("p (h q) -> p h q", h=8),
                    in0=bst[:].rearrange("p (h q) -> p h q", h=8),
                    in1=mB_s[:, v, r * 128:(r + 1) * 128].unsqueeze(1).to_broadcast([128, 8, 128]), op=ALU.mult),
                    reads=[rbst, rmB], writes=[rEB[v]])
        NK = 2
        KTs = [P.sb([64, 8, 512], BF16) for _ in range(NK)]; rKT = [P.res() for _ in range(NK)]
        Vs = [P.sb([128, 4, 520], BF16) for _ in range(NK)]; rV = [P.res() for _ in range(NK)]
        kis = [P.sb([64, 512], BF16) for _ in range(2)]; rki = [P.res() for _ in range(2)]
        QAs = [P.sb([64, 1024], BF16) for _ in range(2)]; rQA = [P.res() for _ in range(2)]
        qis = [P.sb([64, 512], BF16) for _ in range(2)]; rqi = [P.res() for _ in range(2)]
        wis = [P.sb([128, 4], F32) for _ in range(2)]; rwi = [P.res() for _ in range(2)]
        rl = [P.sb([128, 512], F32) for _ in range(4)]; rrl = [P.res() for _ in range(4)]
        Eg = [P.sb([128, 512], BF16) for _ in range(2)]; rEg = [P.res() for _ in range(2)]
        QBs = P.sb([64, 1024], BF16); rQB = P.res()
        KBs = P.sb([64, 5120], BF16); rKB = P.res()
        VBs = P.sb([128, 2600], BF16); rVB = P.res()
        bs = P.sb([128, 8 + NIT + 1], F32); rbs = P.res()
        zc = P.sb([128, CHK], BF16); rzc = P.res()
        czc = P.sb([128, CHK], F16); rczc = P.res()
        tcb = P.sb([128, CHK], BF16); rtcb = P.res()
        onesc = P.sb([128, CHK], BF16); rones_c = P.res()
        chc = P.sb([128, 16], F32); rchc = P.res()
        P.op('pool', lambda e: e.memset(onesc[:], 1.0), writes=[rones_c])
        ig = 0
        ie = 0
        for m in range(nm):
            qs = m % 2
            N = 512 * (m + 1)
            P.dma("sp", QBs[:], QTb[m, :, :], writes=[rQB])
            P.dma("sp", KBs[:], KTb[m, :, :], writes=[rKB])
            P.dma("sp", VBs[:], Vb[m, :, :], writes=[rVB])
            var = 0 if m == 0 else 1
            for r in range(5):
                A.unit(dict(
                    Kd=64,
                    kt=lambda h, r=r: KBs[:, h * 640 + r * 128:h * 640 + (r + 1) * 128],
                    qt=lambda h: QBs[:, h * 128:(h + 1) * 128],
                    v=lambda h, r=r: VBs[:, r * 520 + h * 65:r * 520 + (h + 1) * 65],
                    rk=[rKB], rq=[rQB], rv=[rVB],
                    E=EB[var][:, r * 1024:(r + 1) * 1024].rearrange("p (h q) -> p h q", h=8), rE=[rEB[var]],
                    first=(r == 0), last=(r == 4), ydst=y[m, :, 512:1024]))
            A.flush()
            P.dma("sp", QAs[qs][:], QTa[m, :, :], writes=[rQA[qs]])
            P.dma("sp", qis[qs][:], qiT[m, :, :], writes=[rqi[qs]])
            P.dma("sp", wis[qs][:], wi[m, :, :], writes=[rwi[qs]])
            for g in range(m + 1):
                kk = g % 2
                P.dma("sp", kis[kk][:], kiT[:, g * 512:(g + 1) * 512], writes=[rki[kk]])
                for hh in range(4):
                    bank = A.Sps[hh // 2][:, (hh % 2) * 512:(hh % 2 + 1) * 512]
                    P.op("pe", lambda e, bank=bank, hh=hh, kk=kk, qs=qs: e.matmul(bank, lhsT=qis[qs][:, hh * 128:(hh + 1) * 128], rhs=kis[kk][:],
                                                                          start=True, stop=True),
                         reads=[rqi[qs], rki[kk]], writes=[A.rS[hh // 2]])
                for hh in range(4):
                    bank = A.Sps[hh // 2][:, (hh % 2) * 512:(hh % 2 + 1) * 512]
                    P.op("act", lambda e, bank=bank, hh=hh: e.activation(out=rl[hh][:], in_=bank, func=AF.Relu),
                         reads=[A.rS[hh // 2]], writes=[rrl[hh]])
                scg = sc[:, g * 512:(g + 1) * 512]
                P.op("dve", lambda e, scg=scg, qs=qs: e.tensor_scalar(out=scg, in0=rl[0][:], scalar1=wis[qs][:, 0:1], scalar2=None, op0=ALU.mult),
                     reads=[rrl[0], rwi[qs]], writes=[rsc])
                for hh in range(1, 4):
                    P.op("dve", lambda e, scg=scg, hh=hh, qs=qs: e.scalar_tensor_tensor(out=scg, in0=rl[hh][:], scalar=wis[qs][:, hh:hh + 1], in1=scg,
                                                                                 op0=ALU.mult, op1=ALU.add),
                         reads=[rrl[hh], rwi[qs], rsc], writes=[rsc])
            scN = sc[:, 0:N]
            P.op("dve", lambda e, scN=scN: e.tensor_reduce(out=bs[:, 0:1], in_=scN, axis=AX.X, op=ALU.max), reads=[rsc], writes=[rbs])
            P.op("dve", lambda e, scN=scN: e.tensor_reduce(out=bs[:, 1:2], in_=scN, axis=AX.X, op=ALU.min), reads=[rsc], writes=[rbs])
            P.op("dve", lambda e, m=m: e.tensor_tensor(out=sc[:, m * 512:(m + 1) * 512], in0=sc[:, m * 512:(m + 1) * 512], in1=mA_s[:], op=ALU.add),
                 reads=[rsc, rmA], writes=[rsc])
            P.op("dve", lambda e: e.tensor_tensor(out=bs[:, 2:3], in0=bs[:, 0:1], in1=bs[:, 1:2], op=ALU.subtract), reads=[rbs], writes=[rbs])
            P.op("dve", lambda e: e.scalar_tensor_tensor(out=bs[:, 1:2], in0=bs[:, 2:3], scalar=-0.001, in1=bs[:, 1:2], op0=ALU.mult, op1=ALU.add),
                 reads=[rbs], writes=[rbs])
            P.op("dve", lambda e: e.tensor_scalar(out=bs[:, 1:2], in0=bs[:, 1:2], scalar1=-5e-6, scalar2=None, op0=ALU.add),
                 reads=[rbs], writes=[rbs])
            P.op("dve", lambda e: e.tensor_scalar(out=bs[:, 2:3], in0=bs[:, 2:3], scalar1=1.002, scalar2=1e-5, op0=ALU.mult, op1=ALU.add),
                 reads=[rbs], writes=[rbs])
            P.op("dve", lambda e: e.tensor_scalar(out=bs[:, 8:8 + NIT + 1], in0=ctab[:], scalar1=bs[:, 2:3], scalar2=None, op0=ALU.mult),
                 reads=[rbs, rct], writes=[rbs])
            P.op("dve", lambda e: e.tensor_tensor(out=bs[:, 3:4], in0=bs[:, 1:2], in1=bs[:, 8:9], op=ALU.add), reads=[rbs], writes=[rbs])
            for k in range(NIT):
                P.op("dve", lambda e, scN=scN, N=N: e.tensor_scalar(out=msk[:, 0:N], in0=scN, scalar1=bs[:, 3:4], scalar2=0.0, op0=ALU.is_ge,
                                                                    op1=ALU.add, accum_out=bs[:, 4:5]),
                     reads=[rsc, rbs], writes=[rmsk, rbs])
                P.op("dve", lambda e: e.tensor_scalar(out=bs[:, 5:6], in0=bs[:, 4:5], scalar1=255.5, scalar2=0.5, op0=ALU.is_ge, op1=ALU.subtract),
                     reads=[rbs], writes=[rbs])
                P.op("dve", lambda e, k=k: e.scalar_tensor_tensor(out=bs[:, 3:4], in0=bs[:, 5:6], scalar=bs[:, 8 + k:9 + k], in1=bs[:, 3:4],
                                                                  op0=ALU.mult, op1=ALU.add),
                     reads=[rbs], writes=[rbs])
            P.op("dve", lambda e: e.tensor_tensor(out=bs[:, 6:7], in0=bs[:, 3:4], in1=bs[:, 8 + NIT:9 + NIT], op=ALU.subtract),
                 reads=[rbs], writes=[rbs])
            P.op("dve", lambda e: e.tensor_tensor(out=bs[:, 7:8], in0=bs[:, 3:4], in1=bs[:, 8 + NIT:9 + NIT], op=ALU.add),
                 reads=[rbs], writes=[rbs])
            P.op("dve", lambda e, scN=scN, N=N: e.tensor_scalar(out=msk[:, 0:N], in0=scN, scalar1=bs[:, 6:7], scalar2=None, op0=ALU.is_ge),
                 reads=[rsc, rbs], writes=[rmsk])
            nch = (N + CHK - 1) // CHK
            for c in range(nch):
                c0, c1 = c * CHK, min(N, (c + 1) * CHK)
                L = c1 - c0
                P.op("dve", lambda e, c=c, c0=c0, c1=c1, L=L: e.tensor_scalar(out=zc[:, 0:L], in0=sc[:, c0:c1], scalar1=bs[:, 7:8], scalar2=0.0,
                                                                             op0=ALU.is_ge, op1=ALU.add, accum_out=chc[:, c:c + 1]),
                     reads=[rsc, rbs], writes=[rzc, rchc])
            P.op("dve", lambda e, nch=nch: e.tensor_reduce(out=chc[:, 8:9], in_=chc[:, 0:nch], axis=AX.X, op=ALU.add), reads=[rchc], writes=[rchc])
            P.op("dve", lambda e: e.tensor_scalar(out=chc[:, 9:10], in0=chc[:, 8:9], scalar1=-1.0, scalar2=256.0, op0=ALU.mult, op1=ALU.add),
                 reads=[rchc], writes=[rchc])
            P.op("dve", lambda e: e.memset(chc[:, 10:11], 0.0), writes=[rchc])
            for c in range(nch):
                c0, c1 = c * CHK, min(N, (c + 1) * CHK)
                L = c1 - c0
                P.op("dve", lambda e, c0=c0, c1=c1, L=L: e.scalar_tensor_tensor(out=zc[:, 0:L], in0=sc[:, c0:c1], scalar=bs[:, 7:8], in1=msk[:, c0:c1],
                                                                               op0=ALU.is_lt, op1=ALU.mult),
                     reads=[rsc, rbs, rmsk], writes=[rzc])
                P.op("dve", lambda e, L=L: e.tensor_tensor_scan(out=czc[:, 0:L], data0=onesc[:, 0:L], data1=zc[:, 0:L], initial=chc[:, 10:11],
                                                                op0=ALU.mult, op1=ALU.add),
                     reads=[rzc, rchc, rones_c], writes=[rczc])
                P.op("dve", lambda e, L=L: e.tensor_copy(out=chc[:, 10:11], in_=czc[:, L - 1:L]), reads=[rczc], writes=[rchc])
                P.op("dve", lambda e, L=L: e.scalar_tensor_tensor(out=tcb[:, 0:L], in0=czc[:, 0:L], scalar=chc[:, 9:10], in1=zc[:, 0:L],
                                                                  op0=ALU.is_gt, op1=ALU.mult),
                     reads=[rczc, rchc, rzc], writes=[rtcb])
                P.op("dve", lambda e, c0=c0, c1=c1, L=L: e.tensor_tensor(out=msk[:, c0:c1], in0=msk[:, c0:c1], in1=tcb[:, 0:L], op=ALU.subtract),
                     reads=[rmsk, rtcb], writes=[rmsk])
            if debug and m == dbg_tile:
                P.dma('sp', d_sc[:, :], sc[:, 0:N], reads=[rsc])
                P.dma('sp', d_mk[:, :], msk[:, 0:N], reads=[rmsk])
                P.dma('sp', d_bs[:, :], bs[:], reads=[rbs])
            for g in range(m + 1):
                ks = ig % NK
                ig += 1
                P.dma("sp", KTs[ks][:], KTa[:, :, g * 512:(g + 1) * 512].rearrange("h d s -> d h s"), writes=[rKT[ks]])
                P.dma("sp", Vs[ks][:], Va[g * 4:(g + 1) * 4, :, :].rearrange("b p e -> p b e"), writes=[rV[ks]])
                es = ie % 2
                ie += 1
                for blk in range(4):
                    P.op("pe", lambda e, es=es, blk=blk, g=g: e.transpose(Tps[es][:, blk * 128:(blk + 1) * 128],
                                                                          msk[:, g * 512 + blk * 128:g * 512 + (blk + 1) * 128], id_s[:]),
                         reads=[rmsk, rid], writes=[rT[es]])
                P.op("dve", lambda e, es=es: e.tensor_copy(out=Eg[es][:], in_=Tps[es][:, 0:512]), reads=[rT[es]], writes=[rEg[es]])
                for blk in range(4):
                    A.unit(dict(
                        Kd=64,
                        kt=lambda h, ks=ks, blk=blk: KTs[ks][:, h, blk * 128:(blk + 1) * 128],
                        qt=lambda h, qs=qs: QAs[qs][:, h * 128:(h + 1) * 128],
                        v=lambda h, ks=ks, blk=blk: Vs[ks][:, blk, h * 65:(h + 1) * 65],
                        rk=[rKT[ks]], rq=[rQA[qs]], rv=[rV[ks]],
                        E=Eg[es][:, blk * 128:(blk + 1) * 128].unsqueeze(1).to_broadcast([128, 8, 128]), rE=[rEg[es]],
                        first=(g == 0 and blk == 0), last=(g == m and blk == 3), ydst=y[m, :, 0:512]))
            A.flush()
        P.emit()
    return P


EPS = 1e-6
D = 1024
FF = 2816
TB = 256
NBLK = 17
TH = NBLK * TB


def build_ffn(final):
    P = Prog()
    hT = P.dram_in("hT", [D, TH], F32)
    yT = P.dram_in("yT", [D, TH], BF16)
    Wo_in = P.dram_in("Wo", [128, 8, D], F32)
    g2_in = P.dram_in("g2", [128, 8], F32)
    Wu_in = P.dram_in("Wu", [128, 8, 2 * FF], F32)
    cw_in = P.dram_in("cw", [128, 44, 4], F32)
    Wd_in = P.dram_in("Wd", [128, 22, D], F32)
    go_in = P.dram_in("go", [128, 8], F32)
    hout = P.dram_out("hout", [D, TH - TB], F32)
    hTv = hT.rearrange("(k p) t -> p k t", p=128)
    yTv = yT.rearrange("(k p) t -> p k t", p=128)
    houtv = hout.rearrange("(k p) t -> p k t", p=128)
    with ExitStack() as st:
        P.stack = st
        Wo = P.sb([128, 8, D], BF16); rWo = P.res()
        Wu = P.sb([128, 8, 2 * FF], BF16); rWu = [P.res() for _ in range(8)]
        Wd = P.sb([128, 22, D], BF16); rWd = P.res()
        stage = [P.sb([128, FF // 2], F32) for _ in range(2)]; rstage = [P.res() for _ in range(2)]
        g2 = P.sb([128, 8], F32); rg2 = P.res()
        go = P.sb([128, 8], F32); rgo = P.res()
        cw = P.sb([128, 44, 4], F32); rcw = P.res()
        ones = P.sb([128, 128], F32); rones = P.res()
        tails = P.sb([128, 44, 2], F32); rtails = P.res()
        P.dma("sp", g2[:], g2_in[:, :], writes=[rg2])
        P.dma("sp", go[:], go_in[:, :], writes=[rgo])
        P.dma("sp", cw[:], cw_in[:, :, :], writes=[rcw])
        P.op("pool", lambda e: e.memset(ones[:], 1.0), writes=[rones])
        P.op("pool", lambda e: e.memset(tails[:], 0.0), writes=[rtails])
        ist = 0

        def load_cast(dst, src, n, rdst, scalar=None):
            nonlocal ist
            s = ist % 2
            ist += 1
            P.dma("sp", stage[s][:, 0:n], src, writes=[rstage[s]])
            eng = "dve" if s == 0 else "pool"
            if scalar is None:
                P.op(eng, lambda e: e.tensor_copy(out=dst, in_=stage[s][:, 0:n]), reads=[rstage[s]], writes=[rdst])
            else:
                P.op(eng, lambda e: e.tensor_scalar(out=dst, in0=stage[s][:, 0:n], scalar1=scalar, scalar2=None, op0=ALU.mult),
                     reads=[rstage[s], rg2], writes=[rdst])

        for k in range(8):
            load_cast(Wo[:, k, :], Wo_in[:, k, :], 1024, rWo)
        for k in range(8):
            for q4 in range(4):
                load_cast(Wu[:, k, q4 * 1408:(q4 + 1) * 1408], Wu_in[:, k, q4 * 1408:(q4 + 1) * 1408], 1408, rWu[k], scalar=g2[:, k:k + 1])
        for c in range(22):
            load_cast(Wd[:, c, :], Wd_in[:, c, :], 1024, rWd)

        hb = P.sb([128, 8, TB], F32); rhb = P.res()
        yb = P.sb([128, 8, TB], BF16); ryb = P.res()
        sq = P.sb([128, 8, TB], F32); rsq = P.res()
        xn = P.sb([128, 8, TB], BF16); rxn = P.res()
        rs = P.sb([128, TB], F32); rrs = P.res()
        act_t = P.sb([128, 22, TB], BF16); ract = P.res()
        ext = [P.sb([128, TB + 2], F32) for _ in range(4)]; rext = [P.res() for _ in range(4)]
        cv = [P.sb([128, TB], F32) for _ in range(4)]; rcv = [P.res() for _ in range(4)]
        ob, rob = hb, rhb
        pA = [P.ps([128, 512], F32) for _ in range(2)]; rpA = [P.res() for _ in range(2)]
        pG = [P.ps([128, 512], F32) for _ in range(4)]; rpG = [P.res() for _ in range(4)]
        pS = P.ps([128, 512], F32); rpS = P.res()
        ia = 0
        ig = 0

        def rms_stats(src, rsrc):
            P.op("pool", lambda e: e.tensor_tensor(out=sq[:], in0=src[:], in1=src[:], op=ALU.mult), reads=[rsrc], writes=[rsq])
            for k in range(8):
                P.op("pe", lambda e, k=k: e.matmul(pS[:, 0:TB], lhsT=ones[:], rhs=sq[:, k, :], start=(k == 0), stop=(k == 7)),
                     reads=[rsq, rones], writes=[rpS])
            P.op("act", lambda e: e.activation(out=rs[:], in_=pS[:, 0:TB], func=AF.Sqrt, bias=EPS, scale=1.0 / D),
                 reads=[rpS], writes=[rrs])
            P.op("dve", lambda e: e.reciprocal(out=rs[:], in_=rs[:]), reads=[rrs], writes=[rrs])

        for blk in range(NBLK):
            tsl = slice(blk * TB, (blk + 1) * TB)
            P.dma("sp", hb[:], hTv[:, :, tsl], writes=[rhb])
            P.dma("sp", yb[:], yTv[:, :, tsl], writes=[ryb])
            for d in range(8):
                a = ia % 2
                ia += 1
                for k in range(8):
                    P.op("pe", lambda e, k=k, d=d, a=a: e.matmul(pA[a][:, 0:TB], lhsT=Wo[:, k, d * 128:(d + 1) * 128], rhs=yb[:, k, :],
                                                                  start=(k == 0), stop=(k == 7)),
                         reads=[rWo, ryb], writes=[rpA[a]])
                P.op("dve", lambda e, d=d, a=a: e.tensor_tensor(out=hb[:, d, :], in0=hb[:, d, :], in1=pA[a][:, 0:TB], op=ALU.add),
                     reads=[rpA[a], rhb], writes=[rhb])
            rms_stats(hb, rhb)
            P.op("dve", lambda e: e.tensor_tensor(out=xn[:], in0=hb[:], in1=rs[:].unsqueeze(1).to_broadcast([128, 8, TB]), op=ALU.mult),
                 reads=[rhb, rrs], writes=[rxn])
            for c in range(22):
                gu = []
                for half in range(2):
                    ch = half * 22 + c
                    gi = ig % 4
                    ig += 1
                    col = half * FF + c * 128
                    for k in range(8):
                        P.op("pe", lambda e, k=k, gi=gi, col=col: e.matmul(pG[gi][:, 0:TB], lhsT=Wu[:, k, col:col + 128], rhs=xn[:, k, :],
                                                                          start=(k == 0), stop=(k == 7)),
                             reads=[rWu[k], rxn], writes=[rpG[gi]])
                    ex, cvv = ext[gi], cv[gi]
                    P.op("dve", lambda e, ex=ex, ch=ch: e.tensor_copy(out=ex[:, 0:2], in_=tails[:, ch, :]), reads=[rtails],
                         writes=[rext[gi]])
                    P.op("act", lambda e, ex=ex, gi=gi: e.activation(out=ex[:, 2:TB + 2], in_=pG[gi][:, 0:TB], func=AF.Copy),
                         reads=[rpG[gi]], writes=[rext[gi]])
                    P.op("dve", lambda e, ex=ex, ch=ch: e.tensor_copy(out=tails[:, ch, :], in_=ex[:, TB:TB + 2]), reads=[rext[gi]],
                         writes=[rtails])
                    P.op("dve", lambda e, ex=ex, cvv=cvv, ch=ch: e.tensor_scalar(out=cvv[:], in0=ex[:, 0:TB], scalar1=cw[:, ch, 0:1],
                                                                                 scalar2=cw[:, ch, 3:4], op0=ALU.mult, op1=ALU.add),
                         reads=[rext[gi], rcw], writes=[rcv[gi]])
                    P.op("dve", lambda e, ex=ex, cvv=cvv, ch=ch: e.scalar_tensor_tensor(out=cvv[:], in0=ex[:, 1:TB + 1], scalar=cw[:, ch, 1:2],
                                                                                        in1=cvv[:], op0=ALU.mult, op1=ALU.add),
                         reads=[rext[gi], rcw, rcv[gi]], writes=[rcv[gi]])
                    P.op("dve", lambda e, ex=ex, cvv=cvv, ch=ch: e.scalar_tensor_tensor(out=cvv[:], in0=ex[:, 2:TB + 2], scalar=cw[:, ch, 2:3],
                                                                                        in1=cvv[:], op0=ALU.mult, op1=ALU.add),
                         reads=[rext[gi], rcw, rcv[gi]], writes=[rcv[gi]])
                    gu.append(gi)
                g_i, u_i = gu
                P.op("act", lambda e, g_i=g_i: e.activation(out=cv[g_i][:], in_=cv[g_i][:], func=AF.Silu), reads=[rcv[g_i]],
                     writes=[rcv[g_i]])
                P.op("dve", lambda e, g_i=g_i, u_i=u_i, c=c: e.tensor_tensor(out=act_t[:, c, :], in0=cv[g_i][:], in1=cv[u_i][:], op=ALU.mult),
                     reads=[rcv[g_i], rcv[u_i]], writes=[ract])
            if blk == 0:
                continue
            for d in range(8):
                a = ia % 2
                ia += 1
                for c in range(22):
                    P.op("pe", lambda e, c=c, d=d, a=a: e.matmul(pA[a][:, 0:TB], lhsT=Wd[:, c, d * 128:(d + 1) * 128], rhs=act_t[:, c, :],
                                                                  start=(c == 0), stop=(c == 21)),
                         reads=[rWd, ract], writes=[rpA[a]])
                P.op("dve", lambda e, d=d, a=a: e.tensor_tensor(out=ob[:, d, :], in0=hb[:, d, :], in1=pA[a][:, 0:TB], op=ALU.add),
                     reads=[rpA[a], rhb], writes=[rob])
            if final:
                rms_stats(ob, rob)
                for k in range(8):
                    P.op("dve", lambda e, k=k: e.scalar_tensor_tensor(out=ob[:, k, :], in0=ob[:, k, :], scalar=go[:, k:k + 1], in1=rs[:],
                                                                      op0=ALU.mult, op1=ALU.mult),
                         reads=[rob, rgo, rrs], writes=[rob])
            P.dma("sp", houtv[:, :, (blk - 1) * TB:blk * TB], ob[:], reads=[rob])
        P.emit()
    return P


BF = ml_dtypes.bfloat16
S = 16384; D = 1024; TC = 4096

def rope_tables(S=16384, dim=64):
    inv = (1.0 / (10000.0 ** (np.arange(0, dim, 2, dtype=np.float32) / np.float32(dim)))).astype(np.float32)
    ang = np.arange(S, dtype=np.float32)[:, None] * inv[None, :]
    return np.cos(ang).astype(np.float32), np.sin(ang).astype(np.float32)

def cs_table():
    c, s = rope_tables()
    return np.concatenate([c, s, c * 0.125, s * 0.125], axis=1).astype(np.float32)

def w_pk(W):
    return np.ascontiguousarray(W.reshape(8, 128, -1).transpose(1, 0, 2))

def g_pk(g):
    return np.ascontiguousarray(g.reshape(8, 128).T)

def proj_inputs(hT_full, layer, norm_g, W, ex):
    cs = cs_table()
    maps = []
    for c in range(8):
        b, qd = c // 4, c % 4
        maps.append(dict(hT=np.ascontiguousarray(hT_full[b][:, qd * TC:(qd + 1) * TC]), g=g_pk(norm_g), W=w_pk(W),
                         cs=np.ascontiguousarray(cs[qd * TC:(qd + 1) * TC]), ex=ex))
    return maps

def ex0(ln_g, ln_b):
    e = np.zeros((128, 128), np.float32); e[:, :64] = ln_g[None]; e[:, 64:] = ln_b[None]; return e

def ex1(fb):
    e = np.zeros((128, 128), np.float32); e[:, :8] = fb[None]; return e

ODD_PERM = np.concatenate([np.arange(0, 1536), np.arange(1544, 2312), np.arange(1536, 1544)])

NM = 32

def split3(F):
    hi = F.astype(BF); r = F - hi.astype(np.float32); mid = r.astype(BF); r2 = r - mid.astype(np.float32); lo = r2.astype(BF)
    return hi, mid, lo

def tiles_of(j):
    return [4 * m + j for m in range(NM)]

def qT_tiles(q, j, H):
    qq = q.reshape(S // 128, 128, H, 64)[tiles_of(j)]
    return np.ascontiguousarray(qq.transpose(0, 3, 2, 1).reshape(NM, 64, H * 128))

def kT_full(k, H):
    return np.ascontiguousarray(k.reshape(S, H, 64).transpose(1, 2, 0))

def v_ext(v, H):
    vv = np.ones((S, H, 65), dtype=v.dtype)
    vv[:, :, :64] = v.reshape(S, H, 64)
    return np.ascontiguousarray(vv.reshape(S // 128, 128, H * 65))

def mask_C(j):
    sl = np.arange(128)[:, None, None]; blk = np.arange(4)[None, :, None]; tl = np.arange(128)[None, None, :]
    return np.ascontiguousarray((128 * blk + sl <= 128 * j + tl).astype(BF).reshape(128, 512))

def mask_D():
    out = np.zeros((2, 128, 2, 128), np.float32)
    sl = np.arange(128)[:, None]; tl = np.arange(128)[None, :]
    for r in range(2):
        sk = (r - 1) * 128 + sl
        cdiff = tl // 64 - np.floor_divide(sk, 64)
        valid = (cdiff >= 0) & (cdiff <= 2)
        out[1, :, r, :] = valid
        out[0, :, r, :] = valid & (sk >= 0)
    return np.ascontiguousarray(out.reshape(2, 128, 256).astype(BF))

def attn1_inputs(pb, fo, sinks):
    maps = []
    shared = {}
    for b in range(2):
        p = pb[b]
        KT = np.ones((8, 70, S), dtype=BF)
        KT[:, 0:64] = kT_full(p[:, 512:1024], 8)
        KT[:, 64:67] = fo[b][:, 3:6]
        kd = p[:, 2048:2176].reshape(S, 2, 64); vd = np.ones((S, 2, 65), dtype=BF); vd[:, :, :64] = p[:, 2176:2304].reshape(S, 2, 64)
        kdp = np.concatenate([np.zeros((128, 2, 64), BF), kd], 0)
        vdp = np.concatenate([np.zeros((128, 2, 65), BF), vd], 0)
        shared[b] = dict(KT=KT, Vc=v_ext(p[:, 1024:1536], 8), kdp=kdp, vdp=vdp)
    mD = mask_D()
    for c in range(8):
        b, j = c // 4, c % 4
        p = pb[b]; sh = shared[b]
        QT = np.ones((NM, 70, 1024), dtype=BF)
        QT[:, 0:64] = qT_tiles(p[:, 0:512], j, 8)
        fq = fo[b][:, 0:3].reshape(8, 3, S // 128, 128)[:, :, tiles_of(j)]
        QT[:, 67:70] = fq.transpose(2, 1, 0, 3).reshape(NM, 3, 1024)
        KTd = np.zeros((NM, 64, 512), BF); Vd = np.zeros((NM, 128, 260), BF)
        for m, i in enumerate(tiles_of(j)):
            kw = sh["kdp"][128 * i:128 * i + 256]
            KTd[m] = kw.reshape(2, 128, 2, 64).transpose(3, 2, 0, 1).reshape(64, 512)
            vw = sh["vdp"][128 * i:128 * i + 256]
            Vd[m] = vw.reshape(2, 128, 2, 65).transpose(1, 0, 2, 3).reshape(128, 260)
        maps.append(dict(QTc=QT, KTc=sh["KT"], Vc=sh["Vc"], mC=mask_C(j), QTd=qT_tiles(p[:, 1536:2048], j, 8),
                         KTd=KTd, Vd=Vd, mD=(mD if j == 0 else np.ascontiguousarray(np.stack([mD[1], mD[1]]))), snk=np.ascontiguousarray(np.broadcast_to(sinks[None, :], (128, 8))).astype(np.float32)))
    return maps

TBF = 256

def ffn_inputs(hT_full, yT_full, Wo, g2, Wu, cwv, cbv, Wd, go):
    cw = np.zeros((128, 44, 4), np.float32)
    cw[:, :, 0:3] = cwv.T.reshape(44, 128, 3).transpose(1, 0, 2)
    cw[:, :, 3] = cbv.reshape(44, 128).T
    Wd_pk = np.ascontiguousarray(Wd.reshape(22, 128, 1024).transpose(1, 0, 2))
    common = dict(Wo=w_pk(Wo), g2=g_pk(g2), Wu=w_pk(Wu), cw=cw, Wd=Wd_pk, go=g_pk(go))
    maps = []
    for c in range(8):
        b, qd = c // 4, c % 4
        lo = qd * TC - TBF
        if lo < 0:
            h = np.concatenate([np.zeros((1024, TBF), np.float32), hT_full[b][:, 0:TC]], 1)
            y = np.concatenate([np.zeros((1024, TBF), BF), yT_full[b][:, 0:TC]], 1)
        else:
            h = hT_full[b][:, lo:lo + TC + TBF]; y = yT_full[b][:, lo:lo + TC + TBF]
        maps.append(dict(hT=np.ascontiguousarray(h), yT=np.ascontiguousarray(y), **common))
    return maps


def mask_Aneg(j):
    kk = np.arange(512)[None, :]; tl = np.arange(128)[:, None]
    return np.where(kk // 64 <= 2 * j + tl // 64, 0.0, -1e30).astype(np.float32)


def mask_B(j):
    out = np.zeros((2, 128, 5, 128), np.float32)
    sl = np.arange(128)[:, None]; tl = np.arange(128)[None, :]
    for r in range(5):
        dd = 8 - 2 * r + tl // 64 - sl // 64
        valid = (dd >= 0) & (dd <= 8)
        out[1, :, r, :] = valid
        out[0, :, r, :] = valid & (r >= 4 - j)
    return np.ascontiguousarray(out.reshape(2, 128, 640).astype(BF))


def bias_B(rel_bias):
    sl = np.arange(128)[:, None]; tl = np.arange(128)[None, :]
    out = np.zeros((128, 5, 8, 128), np.float32)
    for r in range(5):
        idx = np.clip(128 * (4 - r) + tl - sl, -256, 256) + 256
        for h in range(8):
            out[:, r, h, :] = rel_bias[h][idx]
    return np.ascontiguousarray(out.reshape(128, 5120))


def attn0_inputs(pb, pf, rel_bias):
    maps = []
    shared = {}
    for b in range(2):
        p = pb[b]
        kb = np.concatenate([np.zeros((512, 8, 64), BF), p[:, 2368:2880].reshape(S, 8, 64)], 0)
        vb = np.ones((S, 8, 65), BF); vb[:, :, :64] = p[:, 2880:3392].reshape(S, 8, 64)
        vb = np.concatenate([np.zeros((512, 8, 65), BF), vb], 0)
        shared[b] = dict(KTa=kT_full(p[:, 512:1024], 8), Va=v_ext(p[:, 1024:1536], 8),
                         kiT=np.ascontiguousarray(p[:, 1792:1856].T), kb=kb, vb=vb)
    bB = bias_B(rel_bias)
    ident = np.eye(128, dtype=np.float32).astype(BF)
    for c in range(8):
        b, j = c // 4, c % 4
        p = pb[b]; sh = shared[b]
        KTb = np.zeros((NM, 64, 5120), BF); Vb = np.zeros((NM, 128, 2600), BF)
        for m, i in enumerate(tiles_of(j)):
            kw = sh["kb"][128 * i:128 * i + 640]
            KTb[m] = kw.reshape(5, 128, 8, 64).transpose(3, 2, 0, 1).reshape(64, 5120)
            vw = sh["vb"][128 * i:128 * i + 640]
            Vb[m] = vw.reshape(5, 128, 8, 65).transpose(1, 0, 2, 3).reshape(128, 2600)
        wi = np.ascontiguousarray(pf[b].reshape(S // 128, 128, 4)[tiles_of(j)])
        maps.append(dict(QTa=qT_tiles(p[:, 0:512], j, 8), KTa=sh["KTa"], Va=sh["Va"], qiT=qT_tiles(p[:, 1536:1792], j, 4),
                         kiT=sh["kiT"], wi=wi, mAneg=mask_Aneg(j), ident=ident, QTb=qT_tiles(p[:, 1856:2368], j, 8),
                         KTb=KTb, Vb=Vb, bB=bB, mB=mask_B(j)))
    return maps


def _gather_tokens(results, key):
    return [np.concatenate([results[b * 4 + q][key] for q in range(4)], 0) for b in range(2)]


def _y_to_T(results):
    out = []
    for b in range(2):
        yf = np.zeros((S // 128, 128, 1024), BF)
        for j in range(4):
            yf[tiles_of(j)] = results[b * 4 + j]["y"]
        out.append(np.ascontiguousarray(yf.reshape(S, 1024).T))
    return np.stack(out)


_PROGS = {}


def _prog(name, fn):
    if name not in _PROGS:
        _PROGS[name] = fn()
    return _PROGS[name]


def _run(P, maps):
    return run_bass_kernel_spmd(P.nc, maps, core_ids=list(range(8))).results


def kernel(x, norm_mix_g, norm_ffn_g, norm_out_g, even_w_in, even_w_out, idx_k_ln_g, idx_k_ln_b, rel_bias,
           odd_w_in, odd_w_out, forget_b, sinks, ffn_w_up, ffn_conv_w, ffn_conv_b, ffn_w_down):
    f = lambda a: np.asarray(a, dtype=np.float32)
    x = f(x)
    hT0 = np.ascontiguousarray(x.transpose(0, 2, 1))
    r = _run(build_proj(0), proj_inputs(hT0, 0, f(norm_mix_g)[0], f(even_w_in)[0], ex0(f(idx_k_ln_g)[0], f(idx_k_ln_b)[0])))
    pb0 = _gather_tokens(r, "pb"); pf0 = _gather_tokens(r, "pf")
    r = _run(build_attn0(), attn0_inputs(pb0, pf0, f(rel_bias)[0]))
    yT = _y_to_T(r)
    r = _run(build_ffn(False), ffn_inputs(hT0, yT, f(even_w_out)[0], f(norm_ffn_g)[0], f(ffn_w_up)[0], f(ffn_conv_w)[0],
                                          f(ffn_conv_b)[0], f(ffn_w_down)[0], f(norm_out_g)))
    hT1 = np.stack([np.concatenate([r[b * 4 + q]["hout"] for q in range(4)], 1) for b in range(2)])
    r = _run(build_proj(1), proj_inputs(hT1, 1, f(norm_mix_g)[1], f(odd_w_in)[0][:, ODD_PERM], ex1(f(forget_b)[0])))
    pb1 = _gather_tokens(r, "pb"); lf = _gather_tokens(r, "pf")
    r = _run(build_fcum(), [dict(lf=np.ascontiguousarray(lf[c // 4].T)) for c in range(8)])
    fo = [r[0]["fo"], r[4]["fo"]]
    r = _run(build_attn1(), attn1_inputs(pb1, fo, f(sinks)[0]))
    yT = _y_to_T(r)
    r = _run(build_ffn(True), ffn_inputs(hT1, yT, f(odd_w_out)[0], f(norm_ffn_g)[1], f(ffn_w_up)[1], f(ffn_conv_w)[1],
                                         f(ffn_conv_b)[1], f(ffn_w_down)[1], f(norm_out_g)))
    oT = np.stack([np.concatenate([r[b * 4 + q]["hout"] for q in range(4)], 1) for b in range(2)])
    return np.ascontiguousarray(oT.transpose(0, 2, 1)).astype(np.float32)
```
